# Optimizing a Trainium2 kernel written in Bass

```python
import jax
import jax.numpy as jnp
from jax import lax
import numpy as np

D_MODEL = 1024
BATCH = 16
SEQ = 2048
DEPTH = 2

GRID_W = 64
CTX_LEN = 256
MLA_HEADS = 8
QK_NOPE = 64
QK_ROPE = 32
V_HEAD = 64
Q_LORA = 256
KV_LORA = 128
Q_BLOCK = 128
CONV_CH = 256
CONV_WIDTH = 31
ML_HEADS = 4
ML_HEAD_DIM = 64
ML_WIDTH = ML_HEADS * ML_HEAD_DIM
MLSTM_CHUNK = 128
D_FF = 4 * D_MODEL
MIX_WIDTH = MLA_HEADS * V_HEAD + CONV_CH + ML_WIDTH
ROPE_THETA = 10000.0
LN_EPS = 1e-5
RMS_EPS = 1e-6
DEEPNORM_ALPHA = (2 * DEPTH) ** 0.25
DEEPNORM_BETA = (8 * DEPTH) ** -0.25
IN_SIZES = (Q_LORA, KV_LORA, QK_ROPE, 2 * CONV_CH, ML_WIDTH, ML_WIDTH, ML_WIDTH, ML_WIDTH, 4 * ML_HEADS)
IN_COLS = sum(IN_SIZES)

kernel_name = "hybrid_mla_conformer_mlstm_dit_block"


def layer_norm(x, g, b):
    xf = x.astype(jnp.float32)
    mu = jnp.mean(xf, -1, keepdims=True)
    var = jnp.mean(jnp.square(xf - mu), -1, keepdims=True)
    return ((xf - mu) * lax.rsqrt(var + LN_EPS) * g + b).astype(x.dtype)


def rms_norm(x, g):
    xf = x.astype(jnp.float32)
    return (xf * lax.rsqrt(jnp.mean(jnp.square(xf), -1, keepdims=True) + RMS_EPS) * g).astype(x.dtype)


def split_cols(p):
    parts, off = [], 0
    for size in IN_SIZES:
        parts.append(p[..., off:off + size])
        off += size
    return parts


def axial_rope(t):
    n = t.shape[-2]
    rows = n // GRID_W
    pos = jnp.arange(rows * GRID_W)
    row, col = pos // GRID_W, pos % GRID_W
    half = QK_ROPE // 2
    nf = half // 2
    inv_freq = ROPE_THETA ** (-jnp.arange(nf, dtype=jnp.float32) / nf)

    def rot(xa, p):
        ang = p.astype(jnp.float32)[:, None] * inv_freq
        cos, sin = jnp.cos(ang), jnp.sin(ang)
        x1 = xa[..., :nf].astype(jnp.float32)
        x2 = xa[..., nf:].astype(jnp.float32)
        return jnp.concatenate([x1 * cos - x2 * sin, x1 * sin + x2 * cos], -1)

    out = jnp.concatenate([rot(t[..., :half], row), rot(t[..., half:], col)], -1)
    return out.astype(t.dtype)


def mla_project(cq, ckv, kr, g_qn, w_uq, g_kvn, w_ukv, with_rope):
    B, L, _ = cq.shape
    q = (rms_norm(cq, g_qn) @ w_uq).reshape(B, L, MLA_HEADS, QK_NOPE + QK_ROPE).transpose(0, 2, 1, 3)
    kv = (rms_norm(ckv, g_kvn) @ w_ukv).reshape(B, L, MLA_HEADS, QK_NOPE + V_HEAD).transpose(0, 2, 1, 3)
    q_nope, q_rope = q[..., :QK_NOPE], q[..., QK_NOPE:]
    k_nope, v = kv[..., :QK_NOPE], kv[..., QK_NOPE:]
    k_rope = kr[:, None]
    if with_rope:
        q_rope = axial_rope(q_rope)
        k_rope = axial_rope(k_rope)
    k_rope = jnp.broadcast_to(k_rope, (B, MLA_HEADS, L, QK_ROPE))
    return (jnp.concatenate([q_nope, q_rope], -1), jnp.concatenate([k_nope, k_rope], -1), v)


def attend(q, k, v):
    scale = (QK_NOPE + QK_ROPE) ** -0.5
    s = jnp.einsum('bhqd,bhkd->bhqk', q, k).astype(jnp.float32) * scale
    p = jax.nn.softmax(s, axis=-1).astype(v.dtype)
    return jnp.einsum('bhqk,bhkd->bhqd', p, v)


def blocked_attend(q, k, v):
    B, H, S, dq = q.shape
    nb = S // Q_BLOCK
    qb = q.reshape(B, H, nb, Q_BLOCK, dq).transpose(2, 0, 1, 3, 4)
    ob = lax.map(lambda qq: attend(qq, k, v), qb)
    return ob.transpose(1, 2, 0, 3, 4).reshape(B, H, S, -1)


def merge_heads(o):
    B, H, L, d = o.shape
    return o.transpose(0, 2, 1, 3).reshape(B, L, H * d)


def conformer_conv(u, w, b, ln_g, ln_b):
    a, gt = u[..., :CONV_CH], u[..., CONV_CH:]
    z = a * jax.nn.sigmoid(gt)
    z = lax.conv_general_dilated(
        z, w[:, None, :].astype(z.dtype), window_strides=(1,),
        padding=[(CONV_WIDTH // 2, CONV_WIDTH // 2)],
        dimension_numbers=('NWC', 'WIO', 'NWC'), feature_group_count=CONV_CH) + b
    return jax.nn.silu(layer_norm(z, ln_g, ln_b))


def mlstm_chunkwise(q, k, v, log_i, log_f, state):
    B, H, L, d = q.shape
    T = MLSTM_CHUNK
    nc = L // T
    tril = jnp.tril(jnp.ones((T, T), dtype=bool))

    def chunks(t):
        return jnp.moveaxis(t.astype(jnp.float32).reshape(t.shape[:2] + (nc, T) + t.shape[3:]), 2, 0)

    def step(carry, inp):
        C, n, m = carry
        qc, kc, vc, lic, lfc = inp
        b = jnp.cumsum(lfc, axis=-1)
        dmat = b[..., :, None] - b[..., None, :] + lic[..., None, :]
        dmat = jnp.where(tril, dmat, -jnp.inf)
        m_inter = b + m[..., None]
        m_t = jnp.maximum(m_inter, jnp.max(dmat, axis=-1))
        wts = jnp.exp(dmat - m_t[..., None])
        inter = jnp.exp(m_inter - m_t)
        sqk = jnp.einsum('bhtd,bhsd->bhts', qc, kc) * wts
        num = jnp.einsum('bhts,bhsd->bhtd', sqk, vc) + inter[..., None] * jnp.einsum('bhvk,bhtk->bhtv', C, qc)
        den = jnp.sum(sqk, -1) + inter * jnp.einsum('bhk,bhtk->bht', n, qc)
        h = num / jnp.maximum(jnp.abs(den), jnp.exp(-m_t))[..., None]
        b_last = b[..., -1]
        g = b_last[..., None] - b + lic
        m_new = jnp.maximum(b_last + m, jnp.max(g, axis=-1))
        w_upd = jnp.exp(g - m_new[..., None])
        decay = jnp.exp(b_last + m - m_new)
        C_new = decay[..., None, None] * C + jnp.einsum('bhs,bhsv,bhsk->bhvk', w_upd, vc, kc)
        n_new = decay[..., None] * n + jnp.einsum('bhs,bhsk->bhk', w_upd, kc)
        return (C_new, n_new, m_new), h

    final, hs = lax.scan(step, state, (chunks(q), chunks(k), chunks(v), chunks(log_i), chunks(log_f)))
    return jnp.moveaxis(hs, 0, 2).reshape(B, H, L, d), final


def to_heads(t):
    B, L, _ = t.shape
    return t.reshape(B, L, ML_HEADS, ML_HEAD_DIM).transpose(0, 2, 1, 3)


def gate_logs(pre, b_gates):
    g = jnp.moveaxis((pre + b_gates).astype(jnp.float32), -1, 1)
    i_f, f_f, i_b, f_b = jnp.split(g, 4, axis=1)
    return i_f, jax.nn.log_sigmoid(f_f), i_b, jax.nn.log_sigmoid(f_b)


def mlstm_bidirectional(q_l, k_l, v_l, gate_l, q_c, k_c, v_c, gate_c, b_gates):
    B = q_l.shape[0]
    ks = ML_HEAD_DIM ** -0.5
    ql, kl, vl = to_heads(q_l), to_heads(k_l) * ks, to_heads(v_l)
    qc, kc, vc = to_heads(q_c), to_heads(k_c) * ks, to_heads(v_c)
    il_f, fl_f, il_b, fl_b = gate_logs(gate_l, b_gates)
    ic_f, fc_f, ic_b, fc_b = gate_logs(gate_c, b_gates)
    zero = (jnp.zeros((B, ML_HEADS, ML_HEAD_DIM, ML_HEAD_DIM), jnp.float32),
            jnp.zeros((B, ML_HEADS, ML_HEAD_DIM), jnp.float32),
            jnp.zeros((B, ML_HEADS), jnp.float32))
    hc_f, st_f = mlstm_chunkwise(qc, kc, vc, ic_f, fc_f, zero)
    hl_f, _ = mlstm_chunkwise(ql, kl, vl, il_f, fl_f, st_f)
    flip = lambda t: jnp.flip(t, axis=2)
    hc_b, st_b = mlstm_chunkwise(flip(qc), flip(kc), flip(vc), flip(ic_b), flip(fc_b), zero)
    hl_b, _ = mlstm_chunkwise(flip(ql), flip(kl), flip(vl), flip(il_b), flip(fl_b), st_b)
    return hl_f + flip(hl_b), hc_f + flip(hc_b)


def mlstm_out(h, o_pre, g):
    B, H, L, d = h.shape
    hh = h.transpose(0, 2, 1, 3) * jax.nn.sigmoid(o_pre.astype(jnp.float32)).reshape(B, L, H, d)
    mu = jnp.mean(hh, -1, keepdims=True)
    var = jnp.mean(jnp.square(hh - mu), -1, keepdims=True)
    hn = (hh - mu) * lax.rsqrt(var + LN_EPS)
    return (hn.reshape(B, L, H * d) * g).astype(o_pre.dtype)


def sqrelu_mlp(u, w1, b1, w2, b2):
    h = jax.nn.relu(u @ w1 + b1)
    return (h * h) @ w2 + b2


def trunk_layer(x, ctx, c, c_ctx, w_ada, b_ada, w_in, g_qn, w_uq, g_kvn, w_ukv,
                conv_w, conv_b, conv_ln_g, conv_ln_b, b_gates, ml_norm_g, w_out,
                ln1_g, ln1_b, w_mlp1, b_mlp1, w_mlp2, b_mlp2, ln2_g, ln2_b, is_last):
    mod = jax.nn.silu(c) @ w_ada + b_ada
    sh1, sc1, g1, sh2, sc2, g2 = jnp.split(mod[:, None, :], 6, axis=-1)
    cmod = jax.nn.silu(c_ctx) @ w_ada + b_ada
    csh1, csc1, cg1, csh2, csc2, cg2 = jnp.split(cmod, 6, axis=-1)

    cq, ckv, kr, conv_in, mq, mk, mv, mo, mg = split_cols((x * (1 + sc1) + sh1) @ w_in)
    ccq, cckv, ckr, cconv_in, cmq, cmk, cmv, cmo, cmg = split_cols((ctx * (1 + csc1) + csh1) @ w_in)

    q_l, k_l, v_l = mla_project(cq, ckv, kr, g_qn, w_uq, g_kvn, w_ukv, True)
    q_c, k_c, v_c = mla_project(ccq, cckv, ckr, g_qn, w_uq, g_kvn, w_ukv, False)
    att = merge_heads(blocked_attend(q_l, jnp.concatenate([k_l, k_c], 2), jnp.concatenate([v_l, v_c], 2)))
    conv = conformer_conv(conv_in, conv_w, conv_b, conv_ln_g, conv_ln_b)
    h_l, h_c = mlstm_bidirectional(mq, mk, mv, mg, cmq, cmk, cmv, cmg, b_gates)
    ml = mlstm_out(h_l, mo, ml_norm_g)

    y = jnp.concatenate([att, conv, ml], -1) @ w_out
    x = layer_norm(DEEPNORM_ALPHA * x + g1 * y, ln1_g, ln1_b)
    x = layer_norm(DEEPNORM_ALPHA * x + g2 * sqrelu_mlp(x * (1 + sc2) + sh2, w_mlp1, b_mlp1, w_mlp2, b_mlp2),
                   ln2_g, ln2_b)

    if not is_last:
        att_c = merge_heads(attend(q_c, k_c, v_c))
        conv_c = conformer_conv(cconv_in, conv_w, conv_b, conv_ln_g, conv_ln_b)
        ml_c = mlstm_out(h_c, cmo, ml_norm_g)
        yc = jnp.concatenate([att_c, conv_c, ml_c], -1) @ w_out
        ctx = layer_norm(DEEPNORM_ALPHA * ctx + cg1 * yc, ln1_g, ln1_b)
        ctx = layer_norm(DEEPNORM_ALPHA * ctx + cg2 * sqrelu_mlp(ctx * (1 + csc2) + csh2, w_mlp1, b_mlp1,
                                                                 w_mlp2, b_mlp2), ln2_g, ln2_b)
    return x, ctx


def setup_inputs(seed: int = 0) -> dict:
    key = jax.random.key(seed)
    ks = iter(jax.random.split(key, 32))

    def nrm(shape, scale):
        return jax.random.normal(next(ks), shape, jnp.float32) * scale

    L = DEPTH
    D = D_MODEL
    x = nrm((BATCH, SEQ, D), 1.0)
    c = nrm((BATCH, D), 1.0)
    ctx = nrm((BATCH, CTX_LEN, D), 1.0)
    c_ctx = nrm((D,), 1.0)
    w_ada = nrm((L, D, 6 * D), D ** -0.5)
    b_ada = nrm((L, 6 * D), 0.02)
    w_in = nrm((L, D, IN_COLS), D ** -0.5)
    g_qn = 1.0 + nrm((L, Q_LORA), 0.05)
    w_uq = nrm((L, Q_LORA, MLA_HEADS * (QK_NOPE + QK_ROPE)), Q_LORA ** -0.5)
    g_kvn = 1.0 + nrm((L, KV_LORA), 0.05)
    w_ukv = nrm((L, KV_LORA, MLA_HEADS * (QK_NOPE + V_HEAD)), KV_LORA ** -0.5)
    conv_w = nrm((L, CONV_WIDTH, CONV_CH), CONV_WIDTH ** -0.5)
    conv_b = nrm((L, CONV_CH), 0.02)
    conv_ln_g = 1.0 + nrm((L, CONV_CH), 0.05)
    conv_ln_b = nrm((L, CONV_CH), 0.02)
    f_bias = jnp.linspace(3.0, 6.0, ML_HEADS)
    z = jnp.zeros((ML_HEADS,), jnp.float32)
    b_gates = jnp.concatenate([z, f_bias, z, f_bias])[None, :] + nrm((L, 4 * ML_HEADS), 0.1)
    ml_norm_g = 1.0 + nrm((L, ML_WIDTH), 0.05)
    w_out = nrm((L, MIX_WIDTH, D), MIX_WIDTH ** -0.5 * DEEPNORM_BETA)
    ln1_g = 1.0 + nrm((L, D), 0.05)
    ln1_b = nrm((L, D), 0.02)
    w_mlp1 = nrm((L, D, D_FF), D ** -0.5)
    b_mlp1 = nrm((L, D_FF), 0.02)
    w_mlp2 = nrm((L, D_FF, D), D_FF ** -0.5 * DEEPNORM_BETA)
    b_mlp2 = nrm((L, D), 0.02)
    ln2_g = 1.0 + nrm((L, D), 0.05)
    ln2_b = nrm((L, D), 0.02)
    return {"x": x, "c": c, "ctx": ctx, "c_ctx": c_ctx, "w_ada": w_ada, "b_ada": b_ada, "w_in": w_in,
            "g_qn": g_qn, "w_uq": w_uq, "g_kvn": g_kvn, "w_ukv": w_ukv, "conv_w": conv_w,
            "conv_b": conv_b, "conv_ln_g": conv_ln_g, "conv_ln_b": conv_ln_b, "b_gates": b_gates,
            "ml_norm_g": ml_norm_g, "w_out": w_out, "ln1_g": ln1_g, "ln1_b": ln1_b, "w_mlp1": w_mlp1,
            "b_mlp1": b_mlp1, "w_mlp2": w_mlp2, "b_mlp2": b_mlp2, "ln2_g": ln2_g, "ln2_b": ln2_b}


def reference(x, c, ctx, c_ctx, w_ada, b_ada, w_in, g_qn, w_uq, g_kvn, w_ukv, conv_w, conv_b,
              conv_ln_g, conv_ln_b, b_gates, ml_norm_g, w_out, ln1_g, ln1_b, w_mlp1, b_mlp1,
              w_mlp2, b_mlp2, ln2_g, ln2_b):
    for l in range(DEPTH):
        x, ctx = trunk_layer(x, ctx, c, c_ctx, w_ada[l], b_ada[l], w_in[l], g_qn[l], w_uq[l], g_kvn[l],
                             w_ukv[l], conv_w[l], conv_b[l], conv_ln_g[l], conv_ln_b[l], b_gates[l],
                             ml_norm_g[l], w_out[l], ln1_g[l], ln1_b[l], w_mlp1[l], b_mlp1[l], w_mlp2[l],
                             b_mlp2[l], ln2_g[l], ln2_b[l], l == DEPTH - 1)
    return x
```

```python
import contextlib
import numpy as np
import concourse.bass as bass
import concourse.mybir as mybir
from concourse.bass_utils import run_bass_kernel_spmd

F32 = mybir.dt.float32
BF16 = mybir.dt.bfloat16
ALU = mybir.AluOpType
AF = mybir.ActivationFunctionType
AX = mybir.AxisListType

D = 1024
TL = 2048
TC = 256
T = TL + TC
NL = 2
NTT = T // 128
ALPHA = 4.0 ** 0.25
LN_EPS = 1e-5
RMS_EPS = 1e-6
KS = 64.0 ** -0.5
ATT_SCALE = 96.0 ** -0.5
TBS = [(0, 512), (512, 512), (1024, 512), (1536, 512), (2048, 256)]
NV = 193
V_BADA, V_GQN, V_GKVN, V_CW, V_CB, V_CLG, V_CLB, V_MLG = 0, 48, 50, 51, 113, 115, 117, 119
V_L1G, V_L1B, V_B1, V_B2, V_L2G, V_L2B = 121, 129, 137, 169, 177, 185
N_DMA_SEM = 24


class _Op:
    __slots__ = ("eng", "fn", "deps", "signal", "is_dma", "tok", "waits", "pre")


class Sched:
    ENGS = ("pe", "act", "dve", "pool", "sp")

    def __init__(self):
        self.ops = {e: [] for e in self.ENGS}
        self.all = []
        self.lastw = {}
        self.readers = {}
        self.barrier_op = None
        self.epoch_last = {}
        self.epoch_dma = []
        self.frozen = False

    def _new(self, eng, fn, is_dma):
        op = _Op()
        op.eng = eng
        op.fn = fn
        op.deps = {}
        op.signal = False
        op.is_dma = is_dma
        op.tok = None
        op.waits = []
        op.pre = None
        return op

    def add(self, eng, fn, reads=(), writes=(), dma=False):
        if self.frozen:
            return None
        op = self._new(eng, fn, dma)
        for k in reads:
            w = self.lastw.get(k)
            if w is not None:
                op.deps[id(w)] = (w, "raw")
        for k in writes:
            w = self.lastw.get(k)
            if w is not None:
                op.deps[id(w)] = (w, "waw")
            for r in self.readers.get(k, ()):
                if id(r) not in op.deps:
                    op.deps[id(r)] = (r, "war")
        for k in reads:
            self.readers.setdefault(k, []).append(op)
        for k in writes:
            self.lastw[k] = op
            self.readers[k] = []
        op.deps.pop(id(op), None)
        if self.barrier_op is not None:
            op.deps[id(self.barrier_op)] = (self.barrier_op, "raw")
        self.ops[eng].append(op)
        self.all.append(op)
        if dma:
            self.epoch_dma.append(op)
        else:
            self.epoch_last[eng] = op
        return op

    def barrier(self, fn):
        if self.frozen:
            return
        op = self._new("pool", fn, False)
        for o in self.epoch_last.values():
            op.deps[id(o)] = (o, "raw")
        for o in self.epoch_dma:
            op.deps[id(o)] = (o, "raw")
        if self.barrier_op is not None:
            op.deps[id(self.barrier_op)] = (self.barrier_op, "raw")
        self.ops["pool"].append(op)
        self.all.append(op)
        self.barrier_op = op
        self.epoch_last = {"pool": op}
        self.epoch_dma = []
        self.lastw = {}
        self.readers = {}

    @staticmethod
    def _needs_sync(op, d, kind):
        if d.is_dma:
            return True
        if d.eng != op.eng:
            return True
        if op.is_dma:
            return True
        if op.eng == "pe":
            return False
        return kind != "war"

    def finalize(self, sem_alloc):
        for op in self.all:
            for d, kind in op.deps.values():
                if self._needs_sync(op, d, kind):
                    d.signal = True
        eng_sem = {}
        cnt = {}
        dsem = {}
        ndma = {}
        for op in self.all:
            if op.is_dma:
                q = op.eng
                lst = dsem.setdefault(q, [])
                nq = ndma.get(q, 0)
                i = nq % N_DMA_SEM
                r = nq // N_DMA_SEM
                if len(lst) <= i:
                    lst.append(sem_alloc("dq_%s%d" % (q, i)))
                op.tok = (lst[i], 16 * (r + 1))
                if r > 0:
                    op.pre = (lst[i], 16 * r)
                ndma[q] = nq + 1
            elif op.signal:
                e = op.eng
                if e not in eng_sem:
                    eng_sem[e] = sem_alloc("e_" + e)
                    cnt[e] = 0
                cnt[e] += 1
                op.tok = (eng_sem[e], cnt[e])
        self.final_dma = []
        for q, lst in dsem.items():
            for i, s_ in enumerate(lst):
                n = (ndma[q] - i + N_DMA_SEM - 1) // N_DMA_SEM
                self.final_dma.append((s_, 16 * n))
        waited = {e: {} for e in self.ENGS}
        for op in self.all:
            need = {}
            if op.pre is not None:
                need[id(op.pre[0])] = op.pre
            for d, kind in op.deps.values():
                if self._needs_sync(op, d, kind):
                    s, v = d.tok
                    if need.get(id(s), (None, 0))[1] < v:
                        need[id(s)] = (s, v)
            for sid, (s, v) in need.items():
                if waited[op.eng].get(sid, 0) >= v:
                    continue
                waited[op.eng][sid] = v
                op.waits.append((s, v))

    def play(self, eng_name, e, final=False):
        for op in self.ops[eng_name]:
            for s, v in op.waits:
                e.wait_ge(s, v)
            ins = op.fn(e)
            if op.is_dma:
                ins.then_inc(op.tok[0], 16)
            elif op.signal:
                ins.then_inc(op.tok[0], 1)
        if final:
            for s, v in self.final_dma:
                e.wait_ge(s, v)


class KB:
    def __init__(self, nc, S, st, nb, nl, dbg):
        self.nc, self.S, self.st, self.nb, self.nl, self.dbg = nc, S, st, nb, nl, dbg
        self._rr = 0

    def mm(self, out, lhsT, rhs, start, stop, reads, writes, sg=False):
        self.S.add("pe", lambda e: e.matmul(out, lhsT, rhs, start=start, stop=stop, skip_group_check=sg), reads, writes)

    def tr(self, out, in_, ident, reads, writes):
        self.S.add("pe", lambda e: e.transpose(out, in_, ident), reads, writes)

    def act(self, out, in_, func, reads, writes, scale=1.0, bias=0.0, eng="act"):
        self.S.add("act", lambda e: e.activation(out=out, in_=in_, func=func, bias=bias, scale=scale), reads, writes)

    def cp(self, eng, out, in_, reads, writes):
        if eng == "act":
            self.S.add("act", lambda e: e.activation(out=out, in_=in_, func=AF.Copy), reads, writes)
        else:
            self.S.add(eng, lambda e: e.tensor_copy(out=out, in_=in_), reads, writes)

    def tt(self, eng, out, in0, in1, op, reads, writes):
        self.S.add(eng, lambda e: e.tensor_tensor(out=out, in0=in0, in1=in1, op=op), reads, writes)

    def ts(self, eng, out, in0, s1, op0, reads, writes, s2=None, op1=None):
        if op1 is None:
            self.S.add(eng, lambda e: e.tensor_scalar(out=out, in0=in0, scalar1=s1, scalar2=None, op0=op0), reads, writes)
        else:
            self.S.add(eng, lambda e: e.tensor_scalar(out=out, in0=in0, scalar1=s1, scalar2=s2, op0=op0, op1=op1), reads, writes)

    def stt(self, out, in0, scalar, in1, op0, op1, reads, writes):
        self.S.add("dve", lambda e: e.scalar_tensor_tensor(out=out, in0=in0, scalar=scalar, in1=in1, op0=op0, op1=op1), reads, writes)

    def dma(self, q, out, in_, reads, writes):
        self.S.add(q, lambda e: e.dma_start(out=out, in_=in_), reads, writes, dma=True)

    def memset(self, eng, ap, val, writes):
        self.S.add(eng, lambda e: e.memset(ap, val), (), writes)

    def barrier(self):
        d = self.DUMMY
        self.S.barrier(lambda e: e.memset(d[:, 0:1], 0.0))

    def evac_eng(self):
        self._rr += 1
        return "act" if self._rr % 2 else "dve"

    def stats(self, psm, psq, kpm, kpq, n, eps, M, VAR, RS, w, kp, with_mean=True):
        inv = 1.0 / n
        vk = kp + ("RS" if VAR is RS else "VAR")
        if with_mean:
            self.act(M[:, :w], psm, AF.Copy, [], [kp + "M", kpm], scale=inv)
            self.stt(VAR[:, :w], M[:, :w], -1.0, M[:, :w], ALU.mult, ALU.mult, [kp + "M"], [vk])
            self.stt(VAR[:, :w], psq, inv, VAR[:, :w], ALU.mult, ALU.add, [vk], [vk, kpq])
            self.act(VAR[:, :w], VAR[:, :w], AF.Ln, [vk], [vk], bias=self.eps_ap(eps))
        else:
            self.act(VAR[:, :w], psq, AF.Ln, [], [vk, kpq], scale=inv, bias=self.eps_ap(eps))
        self.act(RS[:, :w], VAR[:, :w], AF.Exp, [vk], [kp + "RS"], scale=-0.5)

    def eps_ap(self, eps):
        return self.EPS[:, self.eps_idx[eps]:self.eps_idx[eps] + 1]


def build_program(nc, S, st, nb, nl, dbg=None, stop=None):
    kb = KB(nc, S, st, nb, nl, dbg)
    _uid = [0]

    def sb(n, s, d, stack=st):
        _uid[0] += 1
        return stack.enter_context(nc.sbuf_tensor("%s_%d" % (n, _uid[0]), s, d))
    dr = lambda n, s, d, kind: nc.dram_tensor(n, s, d, kind=kind)

    xT = dr("xT", [2, D, T], F32, "ExternalInput")
    cTd = dr("cT", [128, 8, 3], F32, "ExternalInput")
    vecd = dr("vec", [128, NL, NV], F32, "ExternalInput")
    bgd = dr("bg", [128, NL, NTT, 16], F32, "ExternalInput")
    cstd = dr("cst", [128, 6, 128], F32, "ExternalInput")
    roped = dr("rope", [32, 2, TL], F32, "ExternalInput")
    w_ada = dr("w_ada", [NL, D, 6 * D], F32, "ExternalInput")
    w_in = dr("w_in", [NL, D, 2000], F32, "ExternalInput")
    w_uq = dr("w_uq", [NL, 256, 1024], F32, "ExternalInput")
    wk = dr("wk", [NL, 128, 512], F32, "ExternalInput")
    wv = dr("wv", [NL, 128, 512], F32, "ExternalInput")
    w_out = dr("w_out", [NL, D, D], F32, "ExternalInput")
    w1 = dr("w1", [NL, D, 4 * D], F32, "ExternalInput")
    w2 = dr("w2", [NL, 4 * D, D], F32, "ExternalInput")
    yT = dr("yT", [2, D, TL], F32, "ExternalOutput")
    dbgT = dr("dbgT", [D, T], F32, "ExternalOutput") if dbg else None
    S_cq = dr("S_cq", [256, T], F32, "Internal")
    S_ckv = dr("S_ckv", [128, T], F32, "Internal")
    S_kr = dr("S_kr", [32, T], F32, "Internal")
    S_krs = dr("S_krs", [32, T], F32, "Internal")
    S_cv = dr("S_cv", [512, T], F32, "Internal")
    S_mq = dr("S_mq", [256, T], BF16, "Internal")
    S_mk = dr("S_mk", [256, T], BF16, "Internal")
    S_mt = dr("S_mt", [T, 784], F32, "Internal")
    S_h = dr("S_h", [4 * D, T], BF16, "Internal")

    X = sb("X", [128, 8, T], F32)
    CST = sb("CST", [128, 6, 128], F32)
    IDB = sb("IDB", [128, 128], BF16)
    ONESB = sb("ONESB", [128, 128], BF16)
    VEC = sb("VEC", [128, NL, NV], F32)
    DER = sb("DER", [128, NL, 3, 6, 8], F32)
    B2G = sb("B2G", [128, NL, 3, 8], F32)
    EPS = sb("EPS", [128, 4], F32)
    DUMMY = sb("DUMMY", [128, 2], F32)
    kb.DUMMY = DUMMY
    kb.EPS = EPS
    kb.eps_idx = {RMS_EPS: 0, LN_EPS: 1, LN_EPS / (ALPHA * ALPHA): 2}
    P = [st.enter_context(nc.psum_tensor("P%d" % i, [128, 512], F32)) for i in range(8)]
    PK = ["ps%d" % i for i in range(8)]
    IDF = CST[:, 0, :]
    ONESF = CST[:, 1, :]
    TRIF = CST[:, 2, :]
    TRIB = CST[:, 3, :]
    MASK = [CST[:, 4, :], CST[:, 5, :]]

    kb.dma("sp", CST[:], cstd.ap(), [], ["CST"])
    kb.dma("sp", VEC[:], vecd.ap(), [], ["VEC"])
    for v, i in kb.eps_idx.items():
        kb.memset("pool", EPS[:, i:i + 1], float(v), ["EPS"])
    kb.cp("dve", IDB[:], IDF, ["CST"], ["IDB"])
    kb.memset("pool", ONESB[:], 1.0, ["ONESB"])

    def vcol(l, c):
        return VEC[:, l, c:c + 1]

    for k in range(8):
        kb.dma("sp", X[:, k, :], xT.ap()[0, k * 128:(k + 1) * 128, :], [], [("X", k, tb) for tb in range(5)])
    with contextlib.ExitStack() as s0:
        CT = sb("CT", [128, 8, 3], F32, s0)
        SC = sb("SC", [128, 8, 3], F32, s0)
        MOD = sb("MOD", [128, NL, 48, 3], F32, s0)
        WA = [sb("WA%d" % i, [128, 8, 512], F32, s0) for i in range(2)]
        kb.dma("sp", CT[:], cTd.ap(), [], ["CT"])
        kb.act(SC[:], CT[:], AF.Silu, ["CT"], ["SC"])
        for l in range(nl):
            wv_ = w_ada.ap()[l].rearrange("(k p) n -> p k n", p=128)
            for pc in range(12):
                wa = WA[pc % 2]
                wak = "WA%d" % (pc % 2)
                kb.dma("sp", wa[:], wv_[:, :, pc * 512:(pc + 1) * 512], [], [wak])
                bank = pc % 2
                for n4 in range(4):
                    for k in range(8):
                        kb.mm(P[bank][:, n4 * 3:(n4 + 1) * 3], wa[:, k, n4 * 128:(n4 + 1) * 128], SC[:, k, :],
                              k == 0, k == 7, [wak, "SC"], [PK[bank]])
                for n4 in range(4):
                    n = pc * 4 + n4
                    kb.ts("dve", MOD[:, l, n, :], P[bank][:, n4 * 3:(n4 + 1) * 3], vcol(l, V_BADA + n), ALU.add,
                          ["VEC"], [("MOD", l, n), PK[bank]])
            for j in range(3):
                for q in range(6):
                    src = MOD[:, l, q * 8:(q + 1) * 8, j]
                    dst = DER[:, l, j, q, :]
                    rk = [("MOD", l, n) for n in range(q * 8, (q + 1) * 8)]
                    if q in (0, 3):
                        kb.cp("dve", dst, src, rk, ["DER"])
                    elif q in (1, 4):
                        kb.ts("dve", dst, src, 1.0, ALU.add, rk, ["DER"])
                    else:
                        kb.ts("dve", dst, src, 1.0 / ALPHA, ALU.mult, rk, ["DER"])
                kb.tt("dve", B2G[:, l, j, :], DER[:, l, j, 5, :], VEC[:, l, V_B2:V_B2 + 8], ALU.mult, ["DER", "VEC"], ["B2G"])
    kb.barrier()

    def der(l, j, q, o):
        return DER[:, l, j, q, o:o + 1]

    def stage_end(b, l, k):
        if dbg == (b, l) and stop == k:
            for kk in range(8):
                kb.dma("sp", dbgT.ap()[kk * 128:(kk + 1) * 128, :], X[:, kk, :], [("X", kk, tb) for tb in range(5)], [("dbg", kk)])
            S.frozen = True

    for b in range(nb):
        for k in range(8 if b > 0 else 0):
            kb.dma("sp", X[:, k, :], xT.ap()[b, k * 128:(k + 1) * 128, :], [], [("X", k, tb) for tb in range(5)])
        stage_end(b, 0, 0)
        for l in range(nl):
            last = (l == NL - 1)
            ntb = 4 if last else 5
            ntt_out = 16 if last else 18
            jof = lambda tb: (b if tb < 4 else 2)

            def wout_partial(CATs, ckeys, rows0, WO, wok, stk, load=True):
                nch = len(CATs)
                for c in range(nch if load else 0):
                    kb.dma("pool", WO[:, c, :], w_out.ap()[l, rows0 + c * 128: rows0 + (c + 1) * 128, :], [], [wok])
                for tb in range(ntb):
                    t0, w = TBS[tb]
                    for o in range(8):
                        bank = 4 + (o % 4)
                        for c in range(nch):
                            kb.mm(P[bank][:, :w], WO[:, c, o * 128:(o + 1) * 128], CATs[c][:, t0:t0 + w],
                                  c == 0, c == nch - 1, [wok, (ckeys[c], tb)], [PK[bank]])
                        kb.stt(X[:, o, t0:t0 + w], P[bank][:, :w], der(l, jof(tb), 2, o), X[:, o, t0:t0 + w],
                               ALU.mult, ALU.add, ["DER"], [("X", o, tb), PK[bank]])

            with contextlib.ExitStack() as s1:
                WIN = sb("WIN", [128, 8, 2000], BF16, s1)
                UB = [sb("UB%d" % i, [128, 8, 512], BF16, s1) for i in range(2)]
                STG = [sb("STG%d" % i, [128, 512], F32, s1) for i in range(4)]
                STGB = [sb("STGB%d" % i, [128, 512], BF16, s1) for i in range(2)]
                STT = [sb("STT%d" % i, [128, 784], F32, s1) for i in range(2)]
                winv = w_in.ap()[l].rearrange("(k p) c -> p k c", p=128)
                NWP = 8

                def wkeys(c0, m):
                    return [("WIN", i) for i in range(c0 // 250, (c0 + m - 1) // 250 + 1)]
                for i in range(NWP):
                    kb.dma("pool", WIN[:, :, i * 250:(i + 1) * 250], winv[:, :, i * 250:(i + 1) * 250], [], [("WIN", i)])
                fm = [(0, 128, S_cq, 0, False), (128, 128, S_cq, 128, False), (256, 128, S_ckv, 0, False),
                      (384, 32, S_kr, 0, False), (1968, 32, S_krs, 0, False)]
                fm += [(416 + i * 128, 128, S_cv, i * 128, False) for i in range(4)]
                fm += [(928 + i * 128, 128, S_mq, i * 128, True) for i in range(2)]
                fm += [(1184 + i * 128, 128, S_mk, i * 128, True) for i in range(2)]
                nst = 0
                nstb = 0
                bankc = 0
                for tb in range(5):
                    t0, w = TBS[tb]
                    ub = UB[tb % 2]
                    ubk = "UB%d" % (tb % 2)
                    j = jof(tb)
                    for k in range(8):
                        kb.act(ub[:, k, :w], X[:, k, t0:t0 + w], AF.Identity, [("X", k, tb), "DER"], [(ubk, k)],
                               scale=der(l, j, 1, k), bias=der(l, j, 0, k))
                    for (c0, m, dst, r0, isb) in fm:
                        bank = bankc % 4
                        bankc += 1
                        for k in range(8):
                            kb.mm(P[bank][:m, :w], WIN[:, k, c0:c0 + m], ub[:, k, :w], k == 0, k == 7,
                                  wkeys(c0, m) + [(ubk, k)], [PK[bank]])
                        if isb:
                            stg = STGB[nstb % 2]
                            sk = "STGB%d" % (nstb % 2)
                            nstb += 1
                        else:
                            stg = STG[nst % 4]
                            sk = "STG%d" % (nst % 4)
                            nst += 1
                        kb.cp(kb.evac_eng(), stg[:m, :w], P[bank][:m, :w], [], [sk, PK[bank]])
                        kb.dma("sp", dst.ap()[r0:r0 + m, t0:t0 + w], stg[:m, :w], [sk], [(dst.name, r0, tb)])
                    for ti in range(w // 128):
                        tt_ = t0 // 128 + ti
                        stt_ = STT[tt_ % 2]
                        sk = "STT%d" % (tt_ % 2)
                        for (c0, n, bank) in ((1184, 512, 4), (1696, 272, 5)):
                            for k in range(8):
                                kb.mm(P[bank][:, :n], ub[:, k, ti * 128:(ti + 1) * 128], WIN[:, k, c0:c0 + n], k == 0, k == 7,
                                      wkeys(c0, n) + [(ubk, k)], [PK[bank]])
                            kb.cp(kb.evac_eng(), stt_[:, c0 - 1184:c0 - 1184 + n], P[bank][:, :n], [], [(sk, c0), PK[bank]])
                        kb.dma("sp", S_mt.ap()[tt_ * 128:(tt_ + 1) * 128, :], stt_[:], [(sk, 1184), (sk, 1696)],
                               [("S_mt", tt_), (sk, 1184), (sk, 1696)])
            kb.barrier()
            stage_end(b, l, 1)

            sAT = contextlib.ExitStack()
            CATC = [sb("CATC%d" % i, [128, T], BF16, sAT) for i in range(4)]
            with contextlib.ExitStack() as s2:
                ROPE = sb("ROPE", [96, TL], F32, s2)
                CQN = sb("CQN", [128, 2, T], BF16, s2)
                CKVN = sb("CKVN", [128, T], BF16, s2)
                KR = sb("KR", [96, T], BF16, s2)
                WUQ = sb("WUQ", [128, 2, 1024], BF16, s2)
                WKs = sb("WKs", [128, 512], BF16, s2)
                WVs = sb("WVs", [128, 512], BF16, s2)
                VP = sb("VP", [128, NTT, 4, 128], BF16, s2)
                PT = [sb("PT%d" % i, [128, 512], BF16, s2) for i in range(4)]
                R1 = sb("R1", [96, 512], F32, s2)
                R2 = sb("R2", [96, 512], F32, s2)
                R2i = sb("R2i", [32, 512], F32, s2)
                RB = [sb("RB%d" % i, [128, 512], F32, s2) for i in range(2)]
                kb.dma("sp", ROPE[64:96, :], roped.ap()[:, 0, :], [], ["ROPEc"])
                kb.dma("sp", ROPE[0:32, :], roped.ap()[:, 1, :], [], ["ROPEs"])
                for k in range(2):
                    kb.dma("pool", WUQ[:, k, :], w_uq.ap()[l, k * 128:(k + 1) * 128, :], [], ["WUQ"])
                kb.dma("pool", WKs[:], wk.ap()[l], [], ["WKs"])
                kb.dma("pool", WVs[:], wv.ap()[l], [], ["WVs"])
                cqv = S_cq.ap().rearrange("(c p) t -> p c t", p=128)
                with contextlib.ExitStack() as s2a:
                    LD = [sb("LD%d" % i, [128, 2 - (i % 2), 512], F32, s2a) for i in range(4)]
                    SQ = [sb("SQ%d" % i, [128, 2 - (i % 2), 512], F32, s2a) for i in range(4)]
                    RS = [sb("RSa%d" % i, [128, 512], F32, s2a) for i in range(4)]
                    for tb in range(5):
                        t0, w = TBS[tb]
                        i0 = 2 * (tb % 2)
                        i1 = i0 + 1
                        b0, b1 = i0, i1
                        ld = LD[i0]
                        kb.dma("sp", ld[:, :, :w], cqv[:, :, t0:t0 + w], [("S_cq", 0, tb), ("S_cq", 128, tb)], ["LD%d" % i0])
                        kb.act(SQ[i0][:, :, :w], ld[:, :, :w], AF.Square, ["LD%d" % i0], ["SQ%d" % i0])
                        for c in range(2):
                            kb.mm(P[b0][:, :w], ONESF, SQ[i0][:, c, :w], c == 0, c == 1, ["CST", "SQ%d" % i0], [PK[b0]])
                        kb.stats(None, P[b0][:, :w], None, PK[b0], 256, RMS_EPS, None, RS[i0], RS[i0], w, "a%d" % i0, with_mean=False)
                        for c in range(2):
                            kb.stt(CQN[:, c, t0:t0 + w], ld[:, c, :w], vcol(l, V_GQN + c), RS[i0][:, :w], ALU.mult, ALU.mult,
                                   ["LD%d" % i0, "VEC", "a%dRS" % i0], [("CQN", tb)])
                        ld = LD[i1]
                        kb.dma("sp", ld[:, 0, :w], S_ckv.ap()[:, t0:t0 + w], [("S_ckv", 0, tb)], ["LD%d" % i1])
                        kb.act(SQ[i1][:, 0, :w], ld[:, 0, :w], AF.Square, ["LD%d" % i1], ["SQ%d" % i1])
                        kb.mm(P[b1][:, :w], ONESF, SQ[i1][:, 0, :w], True, True, ["CST", "SQ%d" % i1], [PK[b1]])
                        kb.stats(None, P[b1][:, :w], None, PK[b1], 128, RMS_EPS, None, RS[i1], RS[i1], w, "a%d" % i1, with_mean=False)
                        kb.stt(CKVN[:, t0:t0 + w], ld[:, 0, :w], vcol(l, V_GKVN), RS[i1][:, :w], ALU.mult, ALU.mult,
                               ["LD%d" % i1, "VEC", "a%dRS" % i1], [("CKVN", tb)])
                        kb.dma("sp", R1[64:96, :w], S_kr.ap()[:, t0:t0 + w], [("S_kr", 0, tb)], ["R1"])
                        if tb < 4:
                            kb.dma("sp", R2i[:, :w], S_krs.ap()[:, t0:t0 + w], [("S_krs", 0, tb)], ["R2i"])
                            kb.tt("dve", R1[64:96, :w], R1[64:96, :w], ROPE[64:96, t0:t0 + w], ALU.mult, ["R1", "ROPEc"], ["R1"])
                            kb.tt("dve", R2[64:96, :w], R2i[:, :w], ROPE[0:32, t0:t0 + w], ALU.mult, ["R2i", "ROPEs"], ["R2"])
                            kb.tt("dve", KR[64:96, t0:t0 + w], R1[64:96, :w], R2[64:96, :w], ALU.add, ["R1", "R2"], [("KR", tb)])
                        else:
                            kb.cp("dve", KR[64:96, t0:t0 + w], R1[64:96, :w], ["R1"], [("KR", tb)])
                    for tt_ in range(NTT):
                        bank = 4 + tt_ % 3
                        kb.mm(P[bank][:, :], CKVN[:, tt_ * 128:(tt_ + 1) * 128], WVs[:], True, True,
                              [("CKVN", tt_ // 4), "WVs"], [PK[bank]])
                        kb.cp(kb.evac_eng(), VP[:, tt_, :, :], P[bank].reshape([128, 4, 128])[:, :, :], [], [("VA", tt_), PK[bank]])
                kb.barrier()
                QH = [sb("QH%d" % i, [96, T], BF16, s2) for i in range(2)]
                KH = [sb("KH%d" % i, [96, T], BF16, s2) for i in range(2)]
                nqtb = 4 if last else 5
                PO = [P[2], P[3]]
                POK = [PK[2], PK[3]]
                PD = [P[4], P[5]]
                PDK = [PK[4], PK[5]]
                cnt = {"s": 0, "po": 0}

                def q_chunk(h, tb):
                    qh = QH[h % 2]
                    qk = "QH%d" % (h % 2)
                    t0, w = TBS[tb]
                    for k in range(2):
                        kb.mm(P[6][:96, :w], WUQ[:, k, h * 96:(h + 1) * 96], CQN[:, k, t0:t0 + w], k == 0, k == 1,
                              ["WUQ", ("CQN", tb)], [PK[6]])
                    if tb < 4:
                        for k in range(2):
                            kb.mm(P[7][:32, :w], WUQ[:, k, 768 + h * 32:768 + (h + 1) * 32], CQN[:, k, t0:t0 + w], k == 0, k == 1,
                                  ["WUQ", ("CQN", tb)], [PK[7]])
                        kb.tt("dve", R1[64:96, :w], P[6][64:96, :w], ROPE[64:96, t0:t0 + w], ALU.mult, ["ROPEc"], ["R1", PK[6]])
                        kb.tt("dve", R2[64:96, :w], P[7][:32, :w], ROPE[0:32, t0:t0 + w], ALU.mult, ["ROPEs"], ["R2", PK[7]])
                        kb.tt("pool", qh[64:96, t0:t0 + w], R1[64:96, :w], R2[64:96, :w], ALU.add, ["R1", "R2"], [(qk, tb, 0)])
                        kb.cp("dve", qh[0:64, t0:t0 + w], P[6][0:64, :w], [], [(qk, tb, 1), PK[6]])
                    else:
                        kb.cp("dve", qh[0:64, t0:t0 + w], P[6][0:64, :w], [], [(qk, tb, 1), PK[6]])
                        kb.cp("dve", qh[64:96, t0:t0 + w], P[6][64:96, :w], [], [(qk, tb, 0), PK[6]])

                def k_chunk(h, tb):
                    kh = KH[h % 2]
                    kk = "KH%d" % (h % 2)
                    t0, w = TBS[tb]
                    kb.mm(P[7][:64, :w], WKs[:, h * 64:(h + 1) * 64], CKVN[:, t0:t0 + w], True, True,
                          ["WKs", ("CKVN", tb)], [PK[7]])
                    kb.cp("dve", kh[0:64, t0:t0 + w], P[7][0:64, :w], [], [(kk, tb, 1), PK[7]])
                    kb.cp("pool", kh[64:96, t0:t0 + w], KR[64:96, t0:t0 + w], [("KR", tb)], [(kk, tb, 0)])

                def proj_list(h):
                    return [(q_chunk, h, tb) for tb in range(nqtb)] + [(k_chunk, h, tb) for tb in range(5)]

                pending = []

                def attn_block(h, qb):
                    qh, kh = QH[h % 2], KH[h % 2]
                    qk, kk = "QH%d" % (h % 2), "KH%d" % (h % 2)
                    q0, qw = TBS[qb]
                    sbs = list(range(NTT)) if qb < 4 else [16, 17]
                    n = len(sbs)
                    pi = cnt["po"] % 2
                    po, pok, pd, pdk = PO[pi], POK[pi], PD[pi], PDK[pi]
                    rb, rbk = RB[pi], "RB%d" % pi
                    cnt["po"] += 1
                    base = cnt["s"]
                    cnt["s"] += n

                    def S_(i):
                        bank = (base + i) % 2
                        sbk = sbs[i]
                        kb.mm(P[bank][:, :qw], kh[0:96, sbk * 128:(sbk + 1) * 128], qh[0:96, q0:q0 + qw], True, True,
                              [(kk, sbk // 4, 0), (kk, sbk // 4, 1), (qk, qb, 0), (qk, qb, 1)], [PK[bank]])

                    S_(0)
                    if n > 1:
                        S_(1)
                    for i in range(n):
                        bank = (base + i) % 2
                        pt, ptk = PT[(base + i) % 4], "PT%d" % ((base + i) % 4)
                        kb.act(pt[:, :qw], P[bank][:, :qw], AF.Exp, [], [ptk, PK[bank]], scale=ATT_SCALE)
                        if i + 2 < n:
                            S_(i + 2)
                        kb.mm(po[:, :qw], VP[:, sbs[i], h // 2, :], pt[:, :qw], i == 0, i == n - 1, [ptk, ("VA", sbs[i])], [pok])
                        kb.mm(pd[:, :qw], ONESB[:], pt[:, :qw], i == 0, i == n - 1, [ptk, "ONESB"], [pdk])
                        if pending and i % 2 == 1:
                            fn_, h_, tb_ = pending.pop(0)
                            fn_(h_, tb_)
                    hp = (h % 2) * 64
                    kb.S.add("dve", lambda e, o=rb[hp:hp + 64, :qw], i_=pd[hp:hp + 64, :qw]: e.reciprocal(out=o, in_=i_), [], [rbk, pdk])
                    kb.tt("dve", CATC[h // 2][hp:hp + 64, q0:q0 + qw], po[hp:hp + 64, :qw], rb[hp:hp + 64, :qw], ALU.mult, [rbk],
                          [("CATC%d" % (h // 2), qb), pok])

                for fn_, h_, tb_ in proj_list(0):
                    fn_(h_, tb_)
                for h in range(8):
                    if h + 1 < 8:
                        pending.extend(proj_list(h + 1))
                    for qb in range(nqtb):
                        attn_block(h, qb)
                    while pending:
                        fn_, h_, tb_ = pending.pop(0)
                        fn_(h_, tb_)
            kb.barrier()
            stage_end(b, l, 2)


            sML = contextlib.ExitStack()
            CAT3 = [sb("CAT3_%d" % i, [128, T], BF16, sML) for i in range(2)]
            with contextlib.ExitStack() as s4:
                H = sb("H", [128, NTT, 256], F32, s4)
                with contextlib.ExitStack() as s5:
                    MQT = sb("MQT", [128, 2, T], BF16, s5)
                    MKT = sb("MKT", [128, 2, T], BF16, s5)
                    MK = sb("MK", [128, NTT, 256], BF16, s5)
                    MVA = sb("MVA", [128, NTT, 4, 65], BF16, s5)
                    GT = sb("GT", [128, NTT, 16], F32, s5)
                    BGT = sb("BGT", [128, NTT, 16], F32, s5)
                    LI = sb("LI", [128, NTT, 8], F32, s5)
                    LF = sb("LF", [128, NTT, 8], F32, s5)
                    Bc = sb("Bc", [128, NTT, 8], F32, s5)
                    BT = sb("BT", [128, NTT, 8], F32, s5)
                    EA = sb("EA", [128, NTT, 8], F32, s5)
                    EB = sb("EB", [128, NTT, 8], F32, s5)
                    EBT = BT
                    GS = sb("GS", [128, NTT, 8], F32, s5)
                    CTS = sb("CTS", [128, 2, 2, 65], F32, s5)
                    CTB = sb("CTB", [128, 2, 2, 65], BF16, s5)
                    Wt = [[sb("W%d_%d" % (d, e_), [128, 2, 128], BF16, s5) for e_ in range(2)] for d in range(2)]
                    VAe = [sb("VAe0", [128, NTT, 4, 65], BF16, s5), MVA]
                    VGb = [sb("VGb%d" % d, [128, NTT, 4, 65], BF16, s5) for d in range(2)]
                    IEB = sb("IEB", [128, NTT, 8], F32, s5)
                    GSk = GS
                    DN = [sb("DN%d" % d, [128, 4], F32, s5) for d in range(2)]
                    FF = [sb("FF%d" % d, [128, 4], F32, s5) for d in range(2)]
                    HT0 = sb("HTm", [128, 4, 64], F32, s5)
                    HT = [HT0, HT0]
                    mtv = S_mt.ap().rearrange("(t p) c -> p t c", p=128)
                    allmt = [("S_mt", t) for t in range(NTT)]
                    kb.dma("sp", MQT[:], S_mq.ap().rearrange("(c p) t -> p c t", p=128),
                           [("S_mq", r, tb) for r in (0, 128) for tb in range(5)], ["MQT"])
                    kb.dma("sp", MKT[:], S_mk.ap().rearrange("(c p) t -> p c t", p=128),
                           [("S_mk", r, tb) for r in (0, 128) for tb in range(5)], ["MKT"])
                    kb.dma("pool", MK[:], mtv[:, :, 0:256], allmt, ["MK"])
                    kb.memset("pool", MVA[:], 1.0, ["MVA"])
                    for h in range(4):
                        kb.dma("pool", MVA[:, :, h, 0:64], mtv[:, :, 256 + h * 64:256 + (h + 1) * 64], allmt + ["MVA"], ["MVA"])
                    kb.dma("sp", GT[:], mtv[:, :, 768:784], allmt, ["GT"])
                    kb.dma("sp", BGT[:], bgd.ap()[:, l], [], ["BGT"])
                    kb.tt("dve", GT[:], GT[:], BGT[:], ALU.add, ["GT", "BGT"], ["GT"])
                    for d in range(2):
                        kb.cp("dve", LI[:, :, d * 4:(d + 1) * 4], GT[:, :, d * 8:d * 8 + 4], ["GT"], [("LI", d)])
                        kb.act(LF[:, :, d * 4:(d + 1) * 4], GT[:, :, d * 8 + 4:d * 8 + 8], AF.Exp, ["GT"], [("LF", d)], scale=-1.0)
                    kb.act(LF[:], LF[:], AF.Ln, [("LF", 0), ("LF", 1)], ["LF"], bias=1.0)
                    kb.ts("dve", LF[:], LF[:], -1.0, ALU.mult, ["LF"], ["LF"])
                    PB = P[6].reshape([128, 64, 8])
                    PBT = P[7].reshape([128, 64, 8])
                    for t in range(NTT):
                        kb.mm(PB[:, t, 0:4], TRIF, LF[:, t, 0:4], True, True, ["CST", "LF"], [PK[6]])
                        kb.mm(PB[:, t, 4:8], TRIB, LF[:, t, 4:8], True, True, ["CST", "LF"], [PK[6]])
                        kb.mm(PBT[:, t, :], ONESF, LF[:, t, :], True, True, ["CST", "LF"], [PK[7]])
                    kb.cp("dve", Bc[:], PB[:, 0:NTT, :], [], ["Bc", PK[6]])
                    kb.cp("dve", BT[:], PBT[:, 0:NTT, :], [], ["BT", PK[7]])
                    kb.tt("dve", EA[:], LI[:], Bc[:], ALU.subtract, [("LI", 0), ("LI", 1), "Bc"], ["EA"])
                    kb.act(EA[:], EA[:], AF.Exp, ["EA"], ["EA"])
                    kb.act(EB[:], Bc[:], AF.Exp, ["Bc"], ["EB"])
                    kb.act(EBT[:], BT[:], AF.Exp, ["BT"], ["EBT", "BT"])
                    kb.tt("dve", GS[:], EA[:], EBT[:], ALU.mult, ["EA", "EBT"], ["GS"])
                    kb.ts("dve", GSk[:], GS[:], KS, ALU.mult, ["GS"], ["GSk", "GS"])
                    kb.act(IEB[:], Bc[:], AF.Exp, ["Bc"], ["IEB"], scale=-1.0)
                    for d in range(2):
                        eabc = bass.AP(EA, d * 4, [[NTT * 8, 128], [8, NTT], [1, 4], [0, 65]])
                        gsbc = bass.AP(GSk, d * 4, [[NTT * 8, 128], [8, NTT], [1, 4], [0, 65]])
                        kb.tt("pool", VGb[d][:], MVA[:], gsbc, ALU.mult, ["MVA", "GSk"], [("VGb", d)])
                        if d == 0:
                            kb.tt("dve", VAe[0][:], MVA[:], eabc, ALU.mult, ["MVA", "EA"], [("VAe", 0)])
                        else:
                            kb.tt("dve", MVA[:], MVA[:], eabc, ALU.mult, ["MVA", "EA"], [("VAe", 1), "MVA"])
                    stage_end(b, l, 31)
                    CTBall = sb("CTBall", [128, 2, NTT, 2, 65], BF16, s5)
                    kb.memset("pool", CTS[:], 0.0, [("CTS", d, c) for d in range(2) for c in range(2)])
                    kb.memset("pool", CTBall[:], 0.0, [("CTBall", d, s_, c) for d in range(2) for s_ in range(NTT) for c in range(2)])
                    order = [[16, 17] + list(range(16)), [17, 16] + list(range(15, -1, -1))]
                    hwritten = set()
                    for t in range(ntt_out, NTT):
                        kb.memset("dve", H[:, t, :], 0.0, [("H", t)])
                    for step in range(NTT - 1):
                        for d in range(2):
                            t = order[d][step]
                            bU = (2, 0)[step % 2] if d == 0 else (5, 3)[step % 2]
                            PSu = P[bU].reshape([128, 4, 128])
                            for h in range(4):
                                c, hp = h // 2, (h % 2) * 64
                                kb.mm(PSu[:, h, 0:65], MK[:, t, c * 128:(c + 1) * 128], VGb[d][:, t, h, :], h == 0, True,
                                      ["MK", ("VGb", d)], [PK[bU]], sg=True)
                            for h in range(4):
                                c, hp = h // 2, (h % 2) * 64
                                kb.stt(CTS[hp:hp + 64, d, c, :], CTS[hp:hp + 64, d, c, :], EBT[hp:hp + 64, t, d * 4 + h:d * 4 + h + 1],
                                       PSu[hp:hp + 64, h, 0:65], ALU.mult, ALU.add, ["EBT"], [("CTS", d, c), PK[bU]])
                            for c in range(2):
                                kb.cp("act", CTBall[:, d, step + 1, c, :], CTS[:, d, c, :], [("CTS", d, c)], [("CTBall", d, step + 1, c)])
                    items = [(step, d) for step in range(NTT) for d in range(2) if order[d][step] < ntt_out]

                    def SW_(step, d):
                        t = order[d][step]
                        bS, bS2 = 3 * d, 6 + d
                        PSs = [P[bS], P[bS2]]
                        PSk = [PK[bS], PK[bS2]]
                        ts_ = slice(t * 128, (t + 1) * 128)
                        for h in range(4):
                            c, hp = h // 2, (h % 2) * 64
                            kb.mm(PSs[h % 2][:, c * 128:(c + 1) * 128], MKT[hp:hp + 64, c, ts_], MQT[hp:hp + 64, c, ts_], True, True,
                                  ["MKT", "MQT"], [PSk[h % 2]])
                        for e_ in range(2):
                            kb.tt("dve", Wt[d][e_][:], PSs[e_].reshape([128, 4, 128])[:, 0:2, :],
                                  bass.AP(CST, (4 + d) * 128, [[6 * 128, 128], [0, 2], [1, 128]]), ALU.mult,
                                  ["CST"], [("W", d, e_), PSk[e_]])

                    def ODH_(step, d):
                        t = order[d][step]
                        bO = 3 * d + 1
                        PSo = P[bO].reshape([128, 4, 128])
                        ts_ = slice(t * 128, (t + 1) * 128)
                        for h in range(4):
                            c, hp = h // 2, (h % 2) * 64
                            kb.mm(PSo[:, h, 0:65], Wt[d][h % 2][:, c, :], VAe[d][:, t, h, :], h == 0, False,
                                  [("W", d, h % 2), ("VAe", d)], [PK[bO]], sg=True)
                            kb.mm(PSo[:, h, 0:65], MQT[hp:hp + 64, c, ts_], CTBall[hp:hp + 64, d, step, c, :], False, True,
                                  ["MQT", ("CTBall", d, step, c)], [PK[bO]], sg=True)
                        iebs = IEB[:, t, d * 4:(d + 1) * 4]
                        dn, ff = DN[d], FF[d]
                        kb.tt("dve", dn[:], PSo[:, :, 64], iebs, ALU.max, ["IEB"], [("DN", d), PK[bO]])
                        kb.stt(ff[:], PSo[:, :, 64], -1.0, dn[:], ALU.mult, ALU.max, [("DN", d)], [("FF", d), PK[bO]])
                        kb.S.add("dve", lambda e, o=ff[:], i=ff[:]: e.reciprocal(out=o, in_=i), [("FF", d)], [("FF", d)])
                        ffbc = bass.AP(ff, 0, [[4, 128], [1, 4], [0, 64]])
                        hs = H.reshape([128, NTT, 4, 64])[:, t, :, :]
                        if t not in hwritten:
                            hwritten.add(t)
                            kb.tt("dve", hs, PSo[:, :, 0:64], ffbc, ALU.mult, [("FF", d)], [("H", t), PK[bO]])
                        else:
                            kb.tt("dve", HT[d][:], PSo[:, :, 0:64], ffbc, ALU.mult, [("FF", d)], ["HTm", PK[bO]])
                            kb.tt("pool", hs, hs, HT[d][:], ALU.add, ["HTm"], [("H", t)])

                    SW_(*items[0])
                    for i_, it in enumerate(items):
                        if i_ + 1 < len(items):
                            SW_(*items[i_ + 1])
                        ODH_(*it)
                kb.barrier()
                stage_end(b, l, 32)
                with contextlib.ExitStack() as s6:
                    MU = sb("MU", [128, NTT * 4], F32, s6)
                    VR = sb("VR", [128, NTT * 4], F32, s6)
                    MLT = sb("MLT", [128, NTT, 256], BF16, s6)
                    SO = sb("SO", [128, NTT, 256], F32, s6)
                    kb.dma("sp", SO[:], S_mt.ap().rearrange("(t p) c -> p t c", p=128)[:, :, 512:768], [("S_mt", t) for t in range(NTT)], ["SO"])
                    kb.act(SO[:], SO[:], AF.Sigmoid, ["SO"], ["SO"])
                    NG = NTT * 4
                    Hf = H.reshape([128, NG, 64])
                    SOf = SO.reshape([128, NG, 64])

                    NGo = NG

                    def bc(tile_):
                        return bass.AP(tile_, 0, [[NG, 128], [1, NGo], [0, 64]])
                    Hv, SOv = Hf[:, 0:NGo, :], SOf[:, 0:NGo, :]
                    MUv, VRv = MU[:, 0:NGo], VR[:, 0:NGo]
                    kb.tt("dve", Hv, Hv, SOv, ALU.mult, [("H", t) for t in range(NTT)] + ["SO"], ["HH"])
                    kb.S.add("dve", lambda e: e.tensor_reduce(out=MUv, in_=Hv, axis=AX.X, op=ALU.add), ["HH"], ["MU"])
                    kb.ts("dve", MUv, MUv, 1.0 / 64, ALU.mult, ["MU"], ["MU"])
                    kb.tt("dve", Hv, Hv, bc(MU), ALU.subtract, ["HH", "MU"], ["HH"])
                    kb.tt("dve", SOv, Hv, Hv, ALU.mult, ["HH"], ["SO"])
                    kb.S.add("dve", lambda e: e.tensor_reduce(out=VRv, in_=SOv, axis=AX.X, op=ALU.add), ["SO"], ["VR"])
                    kb.act(VRv, VRv, AF.Ln, ["VR"], ["VR"], scale=1.0 / 64, bias=kb.eps_ap(LN_EPS))
                    kb.act(VRv, VRv, AF.Exp, ["VR"], ["VR"], scale=-0.5)
                    kb.tt("dve", MLT.reshape([128, NG, 64])[:, 0:NGo, :], Hv, bc(VR), ALU.mult, ["HH", "VR"], ["MLT"])
                    PTB = P[7].bitcast(BF16)
                    PTB2 = P[6].bitcast(BF16)
                    n = 0
                    for tb in range(ntb):
                        t0, w = TBS[tb]
                        for c in range(2):
                            ptb, pk = (PTB, PK[7]) if n % 2 == 0 else (PTB2, PK[6])
                            n += 1
                            for j in range(w // 128):
                                tt_ = t0 // 128 + j
                                kb.tr(ptb[:, j * 128:(j + 1) * 128], MLT[:, tt_, c * 128:(c + 1) * 128], IDB[:], ["MLT", "IDB"], [pk])
                            kb.act(CAT3[c][:, t0:t0 + w], ptb[:, :w], AF.Identity, ["VEC"], [("ML%d" % c, tb), pk], scale=vcol(l, V_MLG + c))
            kb.barrier()
            stage_end(b, l, 4)
            sCM = contextlib.ExitStack()
            CAT2 = [sb("CAT2_%d" % i, [128, T], BF16, sCM) for i in range(2)]
            with contextlib.ExitStack() as s3:
                ZB = sb("ZB", [128, 2, TL + 30], BF16, s3)
                ZBC = sb("ZBC", [128, 2, TC + 30], BF16, s3)
                DG = sb("DG", [128, 2, 31, 128], BF16, s3)
                ACC = sb("ACC", [128, 2, T], F32, s3)
                LDa = [sb("LDa%d" % i, [128, 512], F32, s3) for i in range(2)]
                LDg = [sb("LDg%d" % i, [128, 512], F32, s3) for i in range(2)]
                SQc = sb("SQc", [128, 2, 512], F32, s3)
                Mc = sb("Mc", [128, 512], F32, s3)
                VARc = sb("VARc", [128, 512], F32, s3)
                RSc = sb("RSc", [128, 512], F32, s3)
                Dc = [sb("Dc%d" % i, [128, 512], F32, s3) for i in range(2)]
                kb.memset("pool", ZB[:], 0.0, [("ZB", c, tb) for c in range(2) for tb in range(4)])
                kb.memset("pool", ZBC[:], 0.0, [("ZBC", c) for c in range(2)])
                for c in range(2):
                    for j in range(31):
                        kb.ts("dve", DG[:, c, j, :], IDB[:], vcol(l, V_CW + c * 31 + j), ALU.mult, ["IDB", "VEC"], [("DG", c)])
                n = 0
                for c in range(2):
                    for tb in range(ntb):
                        t0, w = TBS[tb]
                        la, lg = LDa[n % 2], LDg[n % 2]
                        lak, lgk = "LDa%d" % (n % 2), "LDg%d" % (n % 2)
                        n += 1
                        kb.dma("sp", la[:, :w], S_cv.ap()[c * 128:(c + 1) * 128, t0:t0 + w], [("S_cv", c * 128, tb)], [lak])
                        kb.dma("sp", lg[:, :w], S_cv.ap()[256 + c * 128:256 + (c + 1) * 128, t0:t0 + w], [("S_cv", 256 + c * 128, tb)], [lgk])
                        kb.act(lg[:, :w], lg[:, :w], AF.Sigmoid, [lgk], [lgk])
                        if tb < 4:
                            kb.tt("pool", ZB[:, c, 15 + t0:15 + t0 + w], la[:, :w], lg[:, :w], ALU.mult, [lak, lgk],
                                  [("ZB", c, tb)])
                        else:
                            kb.tt("pool", ZBC[:, c, 15:15 + TC], la[:, :w], lg[:, :w], ALU.mult, [lak, lgk], [("ZBC", c)])
                for tb in range(ntb):
                    t0, w = TBS[tb]
                    for c in range(2):
                        bank = c
                        for j in range(31):
                            if tb < 4:
                                rhs = ZB[:, c, t0 + j:t0 + j + w]
                                rk = [("ZB", c, tb)] + ([("ZB", c, tb - 1)] if tb > 0 else []) + ([("ZB", c, tb + 1)] if tb < 3 else [])
                            else:
                                rhs = ZBC[:, c, j:j + w]
                                rk = [("ZBC", c)]
                            kb.mm(P[bank][:, :w], DG[:, c, j, :], rhs, j == 0, j == 30, [("DG", c)] + rk, [PK[bank]])
                        kb.act(ACC[:, c, t0:t0 + w], P[bank][:, :w], AF.Identity, ["VEC"], [("ACC", c, tb), PK[bank]],
                               bias=vcol(l, V_CB + c))
                    kb.act(SQc[:, :, :w], ACC[:, :, t0:t0 + w], AF.Square, [("ACC", 0, tb), ("ACC", 1, tb)], ["SQc"])
                    for c in range(2):
                        kb.mm(P[2][:, :w], ONESF, ACC[:, c, t0:t0 + w], c == 0, c == 1, ["CST", ("ACC", c, tb)], [PK[2]])
                    for c in range(2):
                        kb.mm(P[3][:, :w], ONESF, SQc[:, c, :w], c == 0, c == 1, ["CST", "SQc"], [PK[3]])
                    kb.stats(P[2][:, :w], P[3][:, :w], PK[2], PK[3], 256, LN_EPS, Mc, VARc, RSc, w, "c")
                    for c in range(2):
                        d = Dc[c]
                        dk = "Dc%d" % c
                        kb.tt("dve", d[:, :w], ACC[:, c, t0:t0 + w], Mc[:, :w], ALU.subtract, [("ACC", c, tb), "cM"], [dk])
                        kb.tt("dve", d[:, :w], d[:, :w], RSc[:, :w], ALU.mult, [dk, "cRS"], [dk])
                        kb.act(CAT2[c][:, t0:t0 + w], d[:, :w], AF.Silu, [dk, "VEC"], [("CV%d" % c, tb)],
                               scale=vcol(l, V_CLG + c), bias=vcol(l, V_CLB + c))
                WO8 = sb("WO8", [128, 8, 1024], BF16, s3)
                wout_partial(CATC + CAT2 + CAT3, ["CATC%d" % i for i in range(4)] + ["CV0", "CV1", "ML0", "ML1"], 0, WO8, "WO8", s3)
            kb.barrier()
            stage_end(b, l, 3)
            sCM.close()
            sML.close()
            sAT.close()

            def ln_pass(gcol, bcol, U2, stk, final_out):
                SQF = [sb("SQF%d" % i, [128, 8, 512], F32, stk) for i in range(2)]
                Ml = [sb("Ml%d" % i, [128, 512], F32, stk) for i in range(2)]
                VARl = [sb("VARl%d" % i, [128, 512], F32, stk) for i in range(2)]
                RSl = [sb("RSl%d" % i, [128, 512], F32, stk) for i in range(2)]
                Dl = [sb("Dl%d" % i, [128, 512], F32, stk) for i in range(3)]

                def A_(tb):
                    t0, w = TBS[tb]
                    s_ = tb % 2
                    bm, bq = 2 * s_, 2 * s_ + 1
                    for o in range(8):
                        if o < 3:
                            kb.tt("pool", SQF[s_][:, o, :w], X[:, o, t0:t0 + w], X[:, o, t0:t0 + w], ALU.mult,
                                  [("X", o, tb)], [("SQF", s_, o)])
                        else:
                            kb.act(SQF[s_][:, o, :w], X[:, o, t0:t0 + w], AF.Square, [("X", o, tb)], [("SQF", s_, o)])
                    for o in range(8):
                        kb.mm(P[bm][:, :w], ONESF, X[:, o, t0:t0 + w], o == 0, o == 7, ["CST", ("X", o, tb)], [PK[bm]])
                    for o in range(8):
                        kb.mm(P[bq][:, :w], ONESF, SQF[s_][:, o, :w], o == 0, o == 7, ["CST", ("SQF", s_, o)], [PK[bq]])
                    kb.stats(P[bm][:, :w], P[bq][:, :w], PK[bm], PK[bq], 1024, LN_EPS / (ALPHA * ALPHA), Ml[s_], VARl[s_], RSl[s_], w,
                             "l%d" % s_)

                def B_(tb):
                    t0, w = TBS[tb]
                    s_ = tb % 2
                    j = jof(tb)
                    for o in range(8):
                        d = Dl[o % 3]
                        dk = "Dl%d" % (o % 3)
                        kb.tt("dve", d[:, :w], X[:, o, t0:t0 + w], Ml[s_][:, :w], ALU.subtract, [("X", o, tb), "l%dM" % s_], [dk])
                        kb.tt("dve", d[:, :w], d[:, :w], RSl[s_][:, :w], ALU.mult, [dk, "l%dRS" % s_], [dk])
                        kb.act(X[:, o, t0:t0 + w], d[:, :w], AF.Identity, [dk, "VEC"], [("X", o, tb)],
                               scale=vcol(l, gcol + o), bias=vcol(l, bcol + o))
                        if U2 is not None:
                            kb.act(U2[:, o, t0:t0 + w], X[:, o, t0:t0 + w], AF.Identity, [("X", o, tb), "DER"], [("U2", o, tb)],
                                   scale=der(l, j, 4, o), bias=der(l, j, 3, o))
                        if final_out and tb < 4:
                            kb.dma("sp", yT.ap()[b, o * 128:(o + 1) * 128, t0:t0 + w], X[:, o, t0:t0 + w], [("X", o, tb)], [("yT", o, tb)])

                A_(0)
                for tb in range(ntb):
                    if tb + 1 < ntb:
                        A_(tb + 1)
                    B_(tb)

            with contextlib.ExitStack() as s7:
                U2 = sb("U2", [128, 8, T], BF16, s7)
                with contextlib.ExitStack() as s8:
                    ln_pass(V_L1G, V_L1B, U2, s8, False)
                kb.barrier()
                with contextlib.ExitStack() as s9:
                    W1P = [sb("W1P%d" % i, [128, 8, 256], BF16, s9) for i in range(3)]
                    HT = [sb("HT%d" % i, [128, 512], F32, s9) for i in range(3)]
                    HB = [sb("HB%d" % i, [128, 512], BF16, s9) for i in range(3)]
                    n = 0
                    w1v = w1.ap()[l].rearrange("(k p) n -> p k n", p=128)

                    def w1load(pc):
                        kb.dma("pool", W1P[pc % 3][:], w1v[:, :, pc * 256:(pc + 1) * 256], [], ["W1P%d" % (pc % 3)])
                    w1load(0)
                    w1load(1)
                    for pc in range(16):
                        wp = W1P[pc % 3]
                        wpk = "W1P%d" % (pc % 3)
                        if pc + 2 < 16:
                            w1load(pc + 2)
                        for fi in range(2):
                            f = pc * 2 + fi
                            for tb in range(ntb):
                                t0, w = TBS[tb]
                                bank = n % 4
                                ht, htk = HT[n % 3], "HT%d" % (n % 3)
                                hb, hbk = HB[n % 3], "HB%d" % (n % 3)
                                n += 1
                                for k in range(8):
                                    kb.mm(P[bank][:, :w], wp[:, k, fi * 128:(fi + 1) * 128], U2[:, k, t0:t0 + w], k == 0, k == 7,
                                          [wpk, ("U2", k, tb)], [PK[bank]])
                                kb.act(ht[:, :w], P[bank][:, :w], AF.Relu, ["VEC"], [htk, PK[bank]], bias=vcol(l, V_B1 + f))
                                kb.tt("dve", hb[:, :w], ht[:, :w], ht[:, :w], ALU.mult, [htk], [hbk])
                                kb.dma("sp", S_h.ap()[f * 128:(f + 1) * 128, t0:t0 + w], hb[:, :w], [hbk], [("S_h", f // 16, tb)])
            kb.barrier()
            stage_end(b, l, 5)
            with contextlib.ExitStack() as s10:
                W2s = sb("W2s", [128, 32, 1024], BF16, s10)
                HBK = [sb("HBK%d" % i, [128, 16, 512], BF16, s10) for i in range(2)]
                TMP = [sb("TMP%d" % i, [128, 512], F32, s10) for i in range(3)]
                for k in range(32):
                    kb.dma("pool", W2s[:, k, :], w2.ap()[l, k * 128:(k + 1) * 128, :], [], [("W2s", k)])
                shv = S_h.ap().rearrange("(k p) t -> p k t", p=128)
                n = 0
                for tb in range(ntb):
                    t0, w = TBS[tb]
                    j = jof(tb)
                    for kh in range(2):
                        kb.dma("sp", HBK[kh][:, :, :w], shv[:, kh * 16:(kh + 1) * 16, t0:t0 + w], [("S_h", kh, tb)], ["HBK%d" % kh])
                        for o in range(8):
                            for kk in range(16):
                                k = kh * 16 + kk
                                kb.mm(P[o][:, :w], W2s[:, k, o * 128:(o + 1) * 128], HBK[kh][:, kk, :w], k == 0, k == 31,
                                      [("W2s", k), "HBK%d" % kh], [PK[o]])
                    for o in range(8):
                        tmp, tk = TMP[n % 3], "TMP%d" % (n % 3)
                        n += 1
                        kb.act(tmp[:, :w], P[o][:, :w], AF.Identity, ["DER", "B2G"], [tk, PK[o]],
                               scale=der(l, j, 5, o), bias=B2G[:, l, j, o:o + 1])
                        kb.tt("pool", X[:, o, t0:t0 + w], X[:, o, t0:t0 + w], tmp[:, :w], ALU.add, [tk], [("X", o, tb)])
            kb.barrier()
            stage_end(b, l, 6)
            with contextlib.ExitStack() as s11:
                ln_pass(V_L2G, V_L2B, None, s11, last)
            kb.barrier()
            stage_end(b, l, 7)
            if dbg == (b, l):
                for k in range(8):
                    kb.dma("sp", dbgT.ap()[k * 128:(k + 1) * 128, :], X[:, k, :], [("X", k, tb) for tb in range(5)], [("dbg", k)])


def make_nc(nb=2, nl=NL, dbg=None, stop=None):
    nc = bass.Bass("TRN2", target_bir_lowering=False)
    S = Sched()
    with contextlib.ExitStack() as st:
        build_program(nc, S, st, nb, nl, dbg, stop)

        def sem_alloc(name):
            return st.enter_context(nc.semaphore(name))

        S.finalize(sem_alloc)
        blk = st.enter_context(nc.Block())

        @blk.sync
        def _(e):
            S.play("sp", e, final=True)

        @blk.tensor
        def _(e):
            S.play("pe", e)

        @blk.scalar
        def _(e):
            S.play("act", e)

        @blk.vector
        def _(e):
            S.play("dve", e)

        @blk.gpsimd
        def _(e):
            S.play("pool", e)
    return nc


_PERM = np.array(list(range(8, 16)) + list(range(0, 8)) + list(range(24, 32)) + list(range(16, 24)))


def _consts():
    cst = np.zeros((128, 6, 128), np.float32)
    r = np.arange(128)[:, None]
    t = np.arange(128)[None, :]
    cst[:, 0] = (r == t)
    cst[:, 1] = 1.0
    cst[:, 2] = (r <= t)
    cst[:, 3] = (r >= t)
    cst[:, 4] = (r <= t) * np.float32(KS)
    cst[:, 5] = (r >= t) * np.float32(KS)
    pos = np.arange(TL)
    row, col = pos // 64, pos % 64
    inv_freq = (np.float32(10000.0) ** (-np.arange(8, dtype=np.float32) / np.float32(8))).astype(np.float32)
    rope = np.zeros((32, 2, TL), np.float32)
    for half, p in enumerate((row, col)):
        ang = p.astype(np.float32)[:, None] * inv_freq
        cs, sn = np.cos(ang).astype(np.float32).T, np.sin(ang).astype(np.float32).T
        base = half * 16
        rope[base:base + 8, 0] = cs
        rope[base + 8:base + 16, 0] = cs
        rope[base:base + 8, 1] = -sn
        rope[base + 8:base + 16, 1] = sn
    return cst, rope


def _prep_shared(inp):
    f = lambda a: np.ascontiguousarray(a, dtype=np.float32)
    vec = np.zeros((128, NL, NV), np.float32)
    for l in range(NL):
        v = vec[:, l]
        v[:, V_BADA:V_BADA + 48] = inp["b_ada"][l].reshape(48, 128).T
        v[:, V_GQN:V_GQN + 2] = inp["g_qn"][l].reshape(2, 128).T
        v[:, V_GKVN] = inp["g_kvn"][l]
        v[:, V_CW:V_CW + 62] = inp["conv_w"][l].T.reshape(2, 128, 31).transpose(1, 0, 2).reshape(128, 62)
        v[:, V_CB:V_CB + 2] = inp["conv_b"][l].reshape(2, 128).T
        v[:, V_CLG:V_CLG + 2] = inp["conv_ln_g"][l].reshape(2, 128).T
        v[:, V_CLB:V_CLB + 2] = inp["conv_ln_b"][l].reshape(2, 128).T
        v[:, V_MLG:V_MLG + 2] = inp["ml_norm_g"][l].reshape(2, 128).T
        v[:, V_L1G:V_L1G + 8] = inp["ln1_g"][l].reshape(8, 128).T
        v[:, V_L1B:V_L1B + 8] = inp["ln1_b"][l].reshape(8, 128).T
        v[:, V_B1:V_B1 + 32] = inp["b_mlp1"][l].reshape(32, 128).T
        v[:, V_B2:V_B2 + 8] = inp["b_mlp2"][l].reshape(8, 128).T
        v[:, V_L2G:V_L2G + 8] = inp["ln2_g"][l].reshape(8, 128).T
        v[:, V_L2B:V_L2B + 8] = inp["ln2_b"][l].reshape(8, 128).T
    bg = np.ascontiguousarray(np.broadcast_to(inp["b_gates"][None, :, None, :], (128, NL, NTT, 16)), dtype=np.float32)
    w_in = inp["w_in"]
    w_in_ext = np.concatenate([w_in, w_in[:, :, 384 + _PERM]], axis=2)
    w_uq = inp["w_uq"]
    w_uq2 = np.zeros((NL, 256, 1024), np.float32)
    wk = np.zeros((NL, 128, 512), np.float32)
    wv = np.zeros((NL, 128, 512), np.float32)
    w_ukv = inp["w_ukv"]
    for h in range(8):
        w_uq2[:, :, h * 96:(h + 1) * 96] = w_uq[:, :, h * 96:(h + 1) * 96]
        w_uq2[:, :, 768 + h * 32:768 + (h + 1) * 32] = w_uq[:, :, h * 96 + 64 + _PERM]
        wk[:, :, h * 64:(h + 1) * 64] = w_ukv[:, :, h * 128:h * 128 + 64]
        wv[:, :, h * 64:(h + 1) * 64] = w_ukv[:, :, h * 128 + 64:h * 128 + 128]
    cst, rope = _consts()
    return {"vec": vec, "bg": bg, "cst": cst, "rope": rope, "w_ada": f(inp["w_ada"]), "w_in": f(w_in_ext),
            "w_uq": w_uq2, "wk": wk, "wv": wv, "w_out": f(inp["w_out"]), "w1": f(inp["w_mlp1"]), "w2": f(inp["w_mlp2"])}


def _prep_core(inp, bs):
    x, ctx, c, c_ctx = inp["x"], inp["ctx"], inp["c"], inp["c_ctx"]
    xT = np.stack([np.concatenate([x[b].T, ctx[b].T], axis=1) for b in bs]).astype(np.float32)
    cc = np.stack([c[bs[0]], c[bs[1]], c_ctx])
    cT = np.ascontiguousarray(cc.reshape(3, 8, 128).transpose(2, 1, 0), dtype=np.float32)
    return {"xT": np.ascontiguousarray(xT), "cT": cT}


def kernel(**inputs):
    inp = {k: np.asarray(v) for k, v in inputs.items()}
    n = 8
    shared = _prep_shared(inp)
    in_maps = []
    for i in range(n):
        m = dict(shared)
        m.update(_prep_core(inp, [2 * i, 2 * i + 1]))
        in_maps.append(m)
    nc = make_nc()
    res = run_bass_kernel_spmd(nc, in_maps, core_ids=list(range(n)))
    out = np.empty((16, TL, D), np.float32)
    for i in range(n):
        y = res.results[i]["yT"]
        for bi in range(2):
            out[2 * i + bi] = y[bi].T
    return out
```
